# Optimizing a Trainium2 kernel written in Bass

```python
import math
import jax, jax.numpy as jnp
from jax import lax
import numpy as np

D_MODEL = 1024
BATCH = 4
SEQ = 8192
DEPTH = 1

RET_HEADS = 4
RET_DK = 128
RET_DV = 128
RET_CHUNK = 128
ROPE_BASE = 10000.0
ATT_Q_HEADS = 8
ATT_KV_HEADS = 2
ATT_GROUP = ATT_Q_HEADS // ATT_KV_HEADS
ATT_HEAD_DIM = 64
WINDOW = 128
ATT_BLOCK = 128
NUM_BUCKETS = 32
MAX_DISTANCE = 128
N_EXPERTS = 32
TOP_K = 4
D_FF = D_MODEL
SWIGLU_ALPHA = 1.702
SWIGLU_LIMIT = 7.0
MOE_BLOCK = 128
PLE_DIM = 256
EPS = 1e-5

RET_WIDTH = RET_HEADS * RET_DV
ATT_WIDTH = ATT_Q_HEADS * ATT_HEAD_DIM
MIX_WIDTH = RET_WIDTH + ATT_WIDTH
IN_SIZES = (RET_HEADS * RET_DK, RET_HEADS * RET_DK, RET_HEADS * RET_DV, RET_WIDTH,
            ATT_Q_HEADS * ATT_HEAD_DIM, ATT_KV_HEADS * ATT_HEAD_DIM, ATT_KV_HEADS * ATT_HEAD_DIM)
IN_WIDTH = sum(IN_SIZES)

kernel_name = "hymba_retention_swa_sinks_moe_ple"


def rmsnorm(x, g):
    x32 = x.astype(jnp.float32)
    y = x32 * lax.rsqrt(jnp.mean(x32 * x32, axis=-1, keepdims=True) + EPS)
    return (y * g.astype(jnp.float32)).astype(x.dtype)


def split_cols(u, sizes):
    outs, start = [], 0
    for s in sizes:
        outs.append(u[..., start:start + s])
        start += s
    return outs


def rope(t, positions):
    half = t.shape[-1] // 2
    inv = ROPE_BASE ** (-jnp.arange(half, dtype=jnp.float32) / half)
    ang = positions.astype(jnp.float32)[..., None] * inv
    cos, sin = jnp.cos(ang)[:, :, None, :], jnp.sin(ang)[:, :, None, :]
    t32 = t.astype(jnp.float32)
    t1, t2 = t32[..., :half], t32[..., half:]
    return jnp.concatenate([t1 * cos - t2 * sin, t1 * sin + t2 * cos], axis=-1).astype(t.dtype)


def retention(q, k, v, positions):
    B, S, H, dk = q.shape
    dv = v.shape[-1]
    dt = q.dtype
    q = rope(q, positions)
    k = rope(k, positions) * (dk ** -0.5)
    C = RET_CHUNK
    nc = S // C
    qc = q.reshape(B, nc, C, H, dk)
    kc = k.reshape(B, nc, C, H, dk)
    vc = v.reshape(B, nc, C, H, dv)
    log_g = jnp.log(1.0 - 2.0 ** (-5.0 - jnp.arange(H, dtype=jnp.float32)))
    c = jnp.arange(C)
    diff = (c[:, None] - c[None, :]).astype(jnp.float32)
    dmask = jnp.where(diff >= 0, jnp.exp(jnp.maximum(diff, 0.0)[None] * log_g[:, None, None]), 0.0).astype(dt)
    scores = jnp.einsum('bnchd,bnmhd->bnhcm', qc, kc) * dmask
    inner = jnp.einsum('bnhcm,bnmhe->bnche', scores, vc)
    cf = c.astype(jnp.float32)
    k_decay = jnp.exp((C - 1 - cf)[None, :] * log_g[:, None]).astype(dt)
    q_decay = jnp.exp((cf + 1)[None, :] * log_g[:, None]).astype(dt)
    chunk_decay = jnp.exp(C * log_g).astype(dt)
    kv = jnp.einsum('bnmhd,bnmhe,hm->bnhde', kc, vc, k_decay)

    def step(state, kv_n):
        new = state * chunk_decay[None, :, None, None] + kv_n
        return new, state

    init = jnp.zeros((B, H, dk, dv), kv.dtype)
    _, r_prev = lax.scan(step, init, jnp.moveaxis(kv, 1, 0))
    r_prev = jnp.moveaxis(r_prev, 0, 1)
    cross = jnp.einsum('bnchd,bnhde,hc->bnche', qc, r_prev, q_decay)
    return (inner + cross).reshape(B, S, H, dv)


def t5_bucket(n):
    max_exact = NUM_BUCKETS // 2
    nf = jnp.maximum(n, 1).astype(jnp.float32)
    large = max_exact + (jnp.log(nf / max_exact) / math.log(MAX_DISTANCE / max_exact)
                         * (NUM_BUCKETS - max_exact)).astype(jnp.int32)
    large = jnp.minimum(large, NUM_BUCKETS - 1)
    return jnp.where(n < max_exact, n, large)


def sliding_window_attention(q, k, v, sinks, bias_table):
    B, S, Hq, dh = q.shape
    L = ATT_BLOCK
    nb = S // L
    qb = q.reshape(B, nb, L, ATT_KV_HEADS, ATT_GROUP, dh)

    def band(t):
        tp = jnp.concatenate([jnp.zeros_like(t[:, :L]), t], axis=1)
        tp = tp.reshape(B, nb + 1, L, ATT_KV_HEADS, dh)
        return jnp.concatenate([tp[:, :-1], tp[:, 1:]], axis=2)

    kb, vb = band(k), band(v)
    qi = jnp.arange(L)[:, None]
    ki = jnp.arange(2 * L)[None, :]
    dist = qi + L - ki
    band_ok = (dist >= 0) & (dist < WINDOW)
    key_ok = (jnp.arange(nb)[:, None] * L - L + ki) >= 0
    mask = band_ok[None] & key_ok[:, None, :]
    bucket = t5_bucket(jnp.maximum(dist, 0))
    bias = jnp.transpose(bias_table[bucket], (2, 0, 1)).astype(jnp.float32)
    bias = bias.reshape(ATT_KV_HEADS, ATT_GROUP, L, 2 * L)
    s = jnp.einsum('bnqhgd,bnkhd->bnhgqk', qb, kb).astype(jnp.float32) * (dh ** -0.5) + bias
    s = jnp.where(mask[None, :, None, None], s, jnp.finfo(jnp.float32).min)
    sink = jnp.broadcast_to(sinks.astype(jnp.float32).reshape(1, 1, ATT_KV_HEADS, ATT_GROUP, 1, 1),
                            s.shape[:-1] + (1,))
    probs = jax.nn.softmax(jnp.concatenate([s, sink], axis=-1), axis=-1)[..., :-1].astype(v.dtype)
    o = jnp.einsum('bnhgqk,bnkhd->bnqhgd', probs, vb)
    return o.reshape(B, S, Hq, dh)


def clamped_swiglu(h):
    glu = jnp.minimum(h[..., ::2], SWIGLU_LIMIT)
    lin = jnp.clip(h[..., 1::2], -SWIGLU_LIMIT, SWIGLU_LIMIT)
    return glu * jax.nn.sigmoid(SWIGLU_ALPHA * glu) * (lin + 1.0)


def moe(x, w_router, b_router, w_up, b_up, w_down, b_down):
    B, S, D = x.shape
    N = B * S
    xf = x.reshape(N, D)
    logits = (xf @ w_router + b_router).astype(jnp.float32)
    top_v, top_i = lax.top_k(logits, TOP_K)
    gate = jax.nn.softmax(top_v, axis=-1).astype(x.dtype)
    n_slots = N * TOP_K
    flat_e = top_i.reshape(-1)
    order = jnp.argsort(flat_e, stable=True)
    sorted_e = flat_e[order]
    counts = jnp.bincount(flat_e, length=N_EXPERTS)
    padded = (counts + MOE_BLOCK - 1) // MOE_BLOCK * MOE_BLOCK
    off = jnp.cumsum(counts) - counts
    pend = jnp.cumsum(padded)
    poff = pend - padded
    dest_sorted = poff[sorted_e] + (jnp.arange(n_slots) - off[sorted_e])
    cap = n_slots + N_EXPERTS * MOE_BLOCK
    n_blocks = cap // MOE_BLOCK
    row_token = jnp.full((cap,), N, jnp.int32).at[dest_sorted].set((order // TOP_K).astype(jnp.int32))
    block_e = jnp.minimum(jnp.searchsorted(pend, jnp.arange(n_blocks) * MOE_BLOCK, side='right'),
                          N_EXPERTS - 1)
    x_pad = jnp.concatenate([xf, jnp.zeros((1, D), xf.dtype)], axis=0)
    xb = x_pad[row_token].reshape(n_blocks, MOE_BLOCK, D)

    def expert_block(args):
        xblk, e = args
        h = xblk @ w_up[e] + b_up[e]
        return clamped_swiglu(h) @ w_down[e] + b_down[e]

    yb = lax.map(expert_block, (xb, block_e)).reshape(cap, D)
    slot_dest = jnp.zeros((n_slots,), dest_sorted.dtype).at[order].set(dest_sorted)
    y_slots = yb[slot_dest].reshape(N, TOP_K, D)
    return jnp.einsum('nk,nkd->nd', gate, y_slots).reshape(B, S, D)


def setup_inputs(seed: int = 0) -> dict:
    key = jax.random.key(seed)
    ks = jax.random.split(key, 24)

    def nrm(k, shape, scale):
        return jax.random.normal(k, shape, jnp.float32) * scale

    return {
        "x": nrm(ks[0], (BATCH, SEQ, D_MODEL), 1.0),
        "p": nrm(ks[1], (DEPTH, BATCH, SEQ, PLE_DIM), 1.0),
        "positions": (jnp.arange(SEQ, dtype=jnp.int32)[None, :]
                      + jax.random.randint(ks[2], (BATCH, 1), 0, 1024, jnp.int32)),
        "rel_bias_table": nrm(ks[3], (NUM_BUCKETS, ATT_Q_HEADS), 0.5),
        "g_mix_norm": 1.0 + nrm(ks[4], (DEPTH, D_MODEL), 0.01),
        "w_in": nrm(ks[5], (DEPTH, D_MODEL, IN_WIDTH), D_MODEL ** -0.5),
        "b_in": nrm(ks[6], (DEPTH, IN_WIDTH), 0.01),
        "ret_norm_g": 1.0 + nrm(ks[7], (DEPTH, RET_HEADS, RET_DV), 0.01),
        "att_sinks": nrm(ks[8], (DEPTH, ATT_Q_HEADS), 0.5),
        "w_out": nrm(ks[9], (DEPTH, MIX_WIDTH, D_MODEL), MIX_WIDTH ** -0.5),
        "g_moe_norm": 1.0 + nrm(ks[10], (DEPTH, D_MODEL), 0.01),
        "w_router": nrm(ks[11], (DEPTH, D_MODEL, N_EXPERTS), D_MODEL ** -0.5),
        "b_router": nrm(ks[12], (DEPTH, N_EXPERTS), 0.01),
        "w_up": nrm(ks[13], (DEPTH, N_EXPERTS, D_MODEL, 2 * D_FF), D_MODEL ** -0.5),
        "b_up": nrm(ks[14], (DEPTH, N_EXPERTS, 2 * D_FF), 0.01),
        "w_down": nrm(ks[15], (DEPTH, N_EXPERTS, D_FF, D_MODEL), D_FF ** -0.5),
        "b_down": nrm(ks[16], (DEPTH, N_EXPERTS, D_MODEL), 0.01),
        "w_ple_gate": nrm(ks[17], (DEPTH, D_MODEL, D_MODEL), D_MODEL ** -0.5),
        "w_ple_proj": nrm(ks[18], (DEPTH, PLE_DIM, D_MODEL), PLE_DIM ** -0.5),
        "g_ple_norm": 1.0 + nrm(ks[19], (DEPTH, D_MODEL), 0.01),
        "g_final": 1.0 + nrm(ks[20], (D_MODEL,), 0.01),
    }


def reference(x, p, positions, rel_bias_table, g_mix_norm, w_in, b_in, ret_norm_g, att_sinks,
              w_out, g_moe_norm, w_router, b_router, w_up, b_up, w_down, b_down,
              w_ple_gate, w_ple_proj, g_ple_norm, g_final):
    B, S, _ = x.shape
    h = x
    for i in range(DEPTH):
        hn = rmsnorm(h, g_mix_norm[i])
        u = hn @ w_in[i] + b_in[i]
        rq, rk, rv, rg, aq, ak, av = split_cols(u, IN_SIZES)
        ret = retention(rq.reshape(B, S, RET_HEADS, RET_DK), rk.reshape(B, S, RET_HEADS, RET_DK),
                        rv.reshape(B, S, RET_HEADS, RET_DV), positions)
        ret = rmsnorm(ret, ret_norm_g[i]).reshape(B, S, RET_WIDTH)
        ret = jax.nn.silu(rg) * ret
        att = sliding_window_attention(aq.reshape(B, S, ATT_Q_HEADS, ATT_HEAD_DIM),
                                       ak.reshape(B, S, ATT_KV_HEADS, ATT_HEAD_DIM),
                                       av.reshape(B, S, ATT_KV_HEADS, ATT_HEAD_DIM),
                                       att_sinks[i], rel_bias_table).reshape(B, S, ATT_WIDTH)
        h = h + jnp.concatenate([ret, att], axis=-1) @ w_out[i]
        h = h + moe(rmsnorm(h, g_moe_norm[i]), w_router[i], b_router[i],
                    w_up[i], b_up[i], w_down[i], b_down[i])
        gate = jax.nn.sigmoid(h @ w_ple_gate[i])
        h = h + rmsnorm(gate * (p[i] @ w_ple_proj[i]), g_ple_norm[i])
    return rmsnorm(h, g_final)
```

```python
import contextlib
import math
import numpy as np
import concourse.bass as bass
import concourse.mybir as mybir
from concourse.bass_utils import run_bass_kernel_spmd

F32 = mybir.dt.float32
BF16 = mybir.dt.bfloat16
I32 = mybir.dt.int32
ALU = mybir.AluOpType
AF = mybir.ActivationFunctionType
AX = mybir.AxisListType

NCORE = 8
TOK = 4096
D = 1024
NSUPER = 4
NEXP = 32
O_KA, O_KB, O_QA, O_QB, O_RV, O_RG, O_AQ, O_AKD, O_AV, NCOL = 0, 512, 1024, 1536, 2048, 2560, 3072, 3584, 3840, 3968
GAM = [1.0 - 2.0 ** (-5.0 - h) for h in range(4)]
CD = [g ** 128 for g in GAM]
SINSC = 6.283185
EPOCH = 2000
DMA_EPOCH = 1024


class _Op:
    __slots__ = ("id", "eng", "fn", "deps", "is_dma", "key", "signal", "comp", "ndma")


class Sched:
    ENGS = ("pe", "act", "dve", "pool", "sp")

    def __init__(self, nc):
        self.nc = nc
        self.ops = []
        self.eng_ops = {e: [] for e in self.ENGS}
        self.state = {}

    limit = None

    def op(self, eng, fn, reads=(), writes=(), dma=False, key=None, ndma=1, force=False):
        if self.limit is not None and len(self.ops) >= self.limit and not force and fn is not None:
            d = _Op(); d.id = -1; d.signal = False; d.comp = None
            return d
        o = _Op()
        o.id = len(self.ops); o.eng = eng; o.fn = fn; o.deps = set(); o.is_dma = dma
        o.key = key; o.signal = False; o.comp = None; o.ndma = ndma
        deps = o.deps
        for r in reads:
            st = self.state.get(r)
            if st is not None and st[0] is not None:
                deps.add(st[0])
        for w in writes:
            st = self.state.get(w)
            if st is not None:
                if st[0] is not None:
                    deps.add(st[0])
                deps.update(st[1])
        for r in reads:
            self.state.setdefault(r, [None, []])[1].append(o.id)
        for w in writes:
            self.state[w] = [o.id, []]
        deps.discard(o.id)
        if dma and key is None:
            o.key = ("dma", tuple(writes)[0] if writes else tuple(reads)[0])
        self.ops.append(o)
        self.eng_ops[eng].append(o)
        return o

    def barrier(self):
        last = [lst[-1].id for lst in self.eng_ops.values() if lst and lst[-1].fn is not None]
        dm = [o.id for o in self.ops[getattr(self, "_bar0", 0):] if o.is_dma]
        self._bar0 = len(self.ops)
        for e in self.ENGS:
            if e == "pool" and not self.eng_ops[e]:
                continue
            o = self.op(e, None)
            o.deps.update(last)
            o.deps.update(dm)
            o.deps.discard(o.id)
        self.state = {}

    def emit(self, final_wait_ops=()):
        nc = self.nc
        ops = self.ops
        for o in ops:
            if o.eng == "pe" and not o.is_dma and o.fn is not None:
                o.deps = {d for d in o.deps if not (ops[d].eng == "pe" and not ops[d].is_dma and ops[d].fn is not None)}
        for o in ops:
            for d in o.deps:
                ops[d].signal = True
        for o in final_wait_ops:
            o.signal = True
        eng_cnt = {e: 0 for e in self.ENGS}
        dma_cnt = {}
        for o in ops:
            if not o.signal:
                continue
            if o.is_dma:
                ep, c = dma_cnt.get(o.key, (0, 0))
                if c + 16 * o.ndma > DMA_EPOCH:
                    ep, c = ep + 1, 0
                c += 16 * o.ndma
                dma_cnt[o.key] = (ep, c)
                o.comp = (("dma", o.key, ep), c)
            else:
                c = eng_cnt[o.eng]
                eng_cnt[o.eng] = c + 1
                o.comp = ((o.eng, c // EPOCH), (c % EPOCH) + 1)
        keys, seen = [], set()
        for o in ops:
            if o.comp is not None and o.comp[0] not in seen:
                seen.add(o.comp[0]); keys.append(o.comp[0])
        self.n_sems = len(keys)
        with contextlib.ExitStack() as es:
            sems = {k: es.enter_context(nc.semaphore("s%d" % i)) for i, k in enumerate(keys)}
            block = es.enter_context(nc.Block())
            engmap = {"pe": "tensor", "act": "scalar", "dve": "vector", "pool": "gpsimd", "sp": "sync"}

            def make(ename):
                elist = self.eng_ops[ename]

                def body(eng):
                    waited = {}
                    for o in elist:
                        need = {}
                        for d in o.deps:
                            k, v = ops[d].comp
                            if waited.get(k, 0) >= v:
                                continue
                            if need.get(k, 0) < v:
                                need[k] = v
                        for k, v in need.items():
                            eng.wait_ge(sems[k], v)
                            waited[k] = v
                        if o.fn is None:
                            res = eng.engine_nop() if o.signal else None
                        else:
                            res = o.fn(eng)
                        if o.signal:
                            k, v = o.comp
                            if o.is_dma:
                                assert len(res) == o.ndma, (len(res), o.ndma)
                                for ins in res:
                                    ins.then_inc(sems[k], 16)
                            else:
                                if isinstance(res, (list, tuple)):
                                    res = res[-1]
                                res.then_inc(sems[k], 1)
                    if ename == "sp":
                        for o in final_wait_ops:
                            k, v = o.comp
                            eng.wait_ge(sems[k], v)
                return body

            for ename in self.ENGS:
                if not self.eng_ops[ename] and ename != "sp":
                    continue
                getattr(block, engmap[ename])(make(ename))


def build_program(n_super=NSUPER, n_exp=NEXP, n_prev=32, stop_after=None, limit=None):
    nc = bass.Bass("TRN2", target_bir_lowering=False)

    def din(name, shape, dt=F32):
        return nc.dram_tensor(name, list(shape), dt, kind="ExternalInput").ap()

    x_own = din("x_own", [TOK, D]); x_prev = din("x_prev", [TOK, D])
    pos_own = din("pos_own", [1, TOK], I32); pos_prev = din("pos_prev", [1, TOK], I32)
    pT_d = din("pT", [256, TOK])
    flag_d = din("flag", [128, 2])
    bias_sw_d = din("bias_sw", [128, 8 * 256])
    w_in_d = din("w_in_r", [D, NCOL]); b_in_d = din("b_in_r", [1, NCOL])
    ropec_d = din("ropec", [128, 2])
    dec_d = din("dec", [1, 1024])
    causal_d = din("causal", [128, 128])
    ident_d = din("ident", [128, 128])
    g_mix_d = din("g_mix", [1, D]); g_ret_d = din("g_ret", [1, 512]); sinks_d = din("sinks", [1, 8])
    w_out_d = din("w_out", [D, D]); g_moe_d = din("g_moe", [1, D])
    w_rt_d = din("w_router", [D, 32]); b_rt_d = din("b_router", [1, 32])
    w_up_d = din("w_up_r", [n_exp, D, 2048]); b_up_d = din("b_up_fm", [128, NEXP * 16])
    w_dn_d = din("w_down", [n_exp, D, D]); b_dn_d = din("b_down", [1, NEXP * D])
    w_pg_d = din("w_ple_gate", [D, D]); w_pp_d = din("w_ple_proj", [256, D])
    g_ple_d = din("g_ple", [1, D]); g_fin_d = din("g_final", [1, D])
    out_d = nc.dram_tensor("out", [TOK, D], F32, kind="ExternalOutput").ap()

    S = Sched(nc)
    S.limit = limit
    out_ops = []

    def V(fn, r, w): return S.op("dve", fn, r, w)
    def A(fn, r, w): return S.op("act", fn, r, w)
    def P(fn, r, w): return S.op("pe", fn, r, w)
    def LD(out, in_, w, eng="sp", r=()): return S.op(eng, lambda e: [e.dma_start(out=out, in_=in_)], r, w, dma=True)

    def mm(out, pairs, r, w):
        def fn(e):
            ins = None
            n = len(pairs)
            for i, (l, rr) in enumerate(pairs):
                ins = e.matmul(out, lhsT=l, rhs=rr, start=(i == 0), stop=(i == n - 1))
            return ins
        return P(fn, r, w)

    with contextlib.ExitStack() as top:
        ARENA_BYTES = 207 * 1024
        arena = top.enter_context(nc.sbuf_tensor("arena", [128, ARENA_BYTES // 4], F32))
        parena = top.enter_context(nc.psum_tensor("parena", [128, 4096], F32))
        sb_top = [0]
        ps_top = [0]
        DTS = {F32: 4, BF16: 2, I32: 4}

        def _carve(base, off_b, shape, dt):
            parts = shape[0]
            free = 1
            for d_ in shape[1:]:
                free *= d_
            nb = (free * DTS[dt] + 31) // 32 * 32
            v = base[0:parts, off_b // 4:(off_b + nb) // 4]
            if dt != F32:
                v = v.bitcast(dt)
            v = v[:, 0:free]
            if len(shape) == 3:
                v = v.rearrange("p (a b) -> p a b", a=shape[1])
            elif len(shape) == 4:
                v = v.rearrange("p (a b c) -> p a b c", a=shape[1], b=shape[2])
            elif len(shape) == 5:
                v = v.rearrange("p (a b c d) -> p a b c d", a=shape[1], b=shape[2], c=shape[3])
            return v, nb

        def sb(es, name, shape, dt=F32):
            v, nb = _carve(arena, sb_top[0], list(shape), dt)
            sb_top[0] += nb
            assert sb_top[0] <= ARENA_BYTES, (name, sb_top[0])
            return v

        def ps(es, name, shape, dt=F32):
            v, nb = _carve(parena, ps_top[0], list(shape), dt)
            nb = (nb + 2047) // 2048 * 2048
            ps_top[0] += nb
            assert ps_top[0] <= 16384, (name, ps_top[0])
            return v

        acc = sb(top, "acc", [128, 8, D])
        Sst = sb(top, "Sst", [128, 4, 128]); Sbf = sb(top, "Sbf", [128, 4, 128], BF16)
        kTw = sb(top, "kTw", [128, 2, 2, 3, 128], BF16)
        vsw = sb(top, "vsw", [128, 3, 128], BF16)
        ident = sb(top, "ident", [128, 128]); identb = sb(top, "identb", [128, 128], BF16)
        causal = sb(top, "causal", [128, 128])
        flag = sb(top, "flag", [128, 2]); ropec = sb(top, "ropec", [128, 2])
        dec = sb(top, "dec", [128, 1024])
        biassw = sb(top, "biassw", [128, 8, 256])
        gret = sb(top, "gret", [128, 512]); sinks = sb(top, "sinks", [128, 8])
        brt = sb(top, "brt", [128, 32])
        epst = sb(top, "epst", [128, 1]); onesb = sb(top, "onesb", [1, 128], BF16)
        junk = sb(top, "junk", [128, D], BF16)

        LD(ident[:], ident_d[:, :], ["ident"]); LD(causal[:], causal_d[:, :], ["causal"])
        LD(flag[:], flag_d[:, :], ["flag"]); LD(ropec[:], ropec_d[:, :], ["ropec"])
        LD(dec[:], dec_d[0:1, :].partition_broadcast(128), ["dec"])
        LD(biassw[:].rearrange("p h k -> p (h k)"), bias_sw_d[:, :], ["biassw"])
        LD(gret[:], g_ret_d[0:1, :].partition_broadcast(128), ["gret"])
        LD(sinks[:], sinks_d[0:1, :].partition_broadcast(128), ["sinks"])
        LD(brt[:], b_rt_d[0:1, :].partition_broadcast(128), ["brt"])
        V(lambda e: e.tensor_copy(out=identb[:], in_=ident[:]), ["ident"], ["identb"])
        V(lambda e: e.memset(epst[:], 1e-5), [], ["epst"])
        V(lambda e: e.memset(onesb[:], 1.0), [], ["onesb"])
        V(lambda e: e.memset(Sst[:], 0.0), [], ["Sst"])
        V(lambda e: e.memset(Sbf[:], 0.0), [], ["Sbf"])
        V(lambda e: e.memset(kTw[:], 0.0), [], ["kTw0", "kTw1", "kTw2"])

        def rmsnorm_stats(src_ap, ss_ap, rstd_ap, keys_r, key_ss, key_rstd, n):
            A(lambda e: e.activation(out=junk[:, 0:n], in_=src_ap, func=AF.Square, scale=1.0 / math.sqrt(n), accum_out=ss_ap),
              keys_r, ["junk", key_ss])
            A(lambda e: e.activation(out=rstd_ap, in_=ss_ap, func=AF.Ln, bias=epst[:, 0:1]), [key_ss, "epst"], [key_rstd])
            A(lambda e: e.activation(out=rstd_ap, in_=rstd_ap, func=AF.Exp, scale=-0.5), [key_rstd], [key_rstd])

        sb_mark = sb_top[0]
        W_TOP = ARENA_BYTES - (8 * NCOL * 2 + NCOL * 2 + 8 * D * 2)
        win, _n1 = _carve(arena, W_TOP, [128, 8, NCOL], BF16)
        bin_, _n2 = _carve(arena, W_TOP + _n1, [1, NCOL], BF16)
        wout, _n3 = _carve(arena, W_TOP + _n1 + _n2, [128, 8, D], BF16)
        assert W_TOP + _n1 + _n2 + _n3 <= ARENA_BYTES

        def load_mixer_weights():
            LD(win[:], w_in_d.rearrange("(c p) n -> p c n", p=128), ["win"], eng="pool")
            LD(bin_[:], b_in_d[:, :], ["bin"], eng="pool")
            LD(wout[:], w_out_d.rearrange("(c p) n -> p c n", p=128), ["wout"], eng="pool")
        for st in range(n_super):
            sb_top[0] = sb_mark
            with contextlib.ExitStack() as ms:
                ps_top[0] = 0
                gmix = sb(ms, "gmix", [128, D])
                LD(gmix[:], g_mix_d[0:1, :].partition_broadcast(128), ["gmix"])
                ropeC = sb(ms, "ropeC", [128, 1024]); ropeD = sb(ms, "ropeD", [128, 1024])
                posi = sb(ms, "posi", [128, 1024], I32)
                xt = [sb(ms, "xt%d" % i, [128, D]) for i in range(2)]
                rt1, rt2, rti = xt[0], xt[1], posi
                xn = sb(ms, "xn", [128, D], BF16)
                xnT = sb(ms, "xnT", [128, 8, 128], BF16)
                ss = sb(ms, "ss", [128, 1]); rstd = sb(ms, "rstd", [128, 1])
                ta = sb(ms, "ta", [128, 4, 128]); tb = sb(ms, "tb", [128, 4, 128])
                kTs = [sb(ms, "kT%d" % i, [128, 4, 128], BF16) for i in range(2)]
                qTs = [sb(ms, "qT%d" % i, [128, 4, 128], BF16) for i in range(2)]
                khats = [sb(ms, "khat%d" % i, [128, 4, 128], BF16) for i in range(2)]
                vrets = [sb(ms, "vret%d" % i, [128, 4, 128], BF16) for i in range(2)]
                sws = [sb(ms, "sw%d" % i, [128, 512]) for i in range(2)]
                aqTs = [sb(ms, "aqT%d" % i, [128, 4, 128], BF16) for i in range(2)]
                PTr = sb(ms, "PTr", [128, 4, 128], BF16)
                rss = sb(ms, "rss", [128, 4]); rrs = sb(ms, "rrs", [128, 4])
                rsq = sb(ms, "rsq", [128, 4, 128]); rt = sb(ms, "rt", [128, 4, 128])
                sc = sb(ms, "sc", [128, 4, 256]); Pb = sb(ms, "Pb", [128, 4, 256], BF16)
                rmax = sb(ms, "rmax", [128, 8]); nmax = sb(ms, "nmax", [128, 8]); rsum = sb(ms, "rsum", [128, 8])
                esk = sb(ms, "esk", [128, 8]); rden = sb(ms, "rden", [128, 8]); rden8 = sb(ms, "rden8", [128, 8])
                PTs = sb(ms, "PTs", [128, 8, 128], BF16)
                cat = sb(ms, "cat", [128, D], BF16); catT = sb(ms, "catT", [128, 8, 128], BF16)
                Ba = ps(ms, "Ba", [128, 8, 128], BF16)
                Bb = ps(ms, "Bb", [128, 8, 128], BF16)
                F = [ps(ms, "F%d" % i, [128, 512]) for i in range(6)]

                assert sb_top[0] <= W_TOP, sb_top[0]
                if st == 0:
                    load_mixer_weights()

                def make_rope(pos_ap):
                    LD(posi[:], pos_ap.partition_broadcast(128), ["posi"])
                    V(lambda e: e.tensor_copy(out=rt1[:], in_=posi[:]), ["posi"], ["xt0"])
                    V(lambda e: e.tensor_scalar(out=rt1[:], in0=rt1[:], scalar1=ropec[:, 0:1], scalar2=ropec[:, 1:2],
                                                op0=ALU.mult, op1=ALU.add), ["xt0", "ropec"], ["xt0"])
                    V(lambda e: e.tensor_copy(out=rti[:], in_=rt1[:]), ["xt0"], ["posi"])
                    V(lambda e: e.tensor_copy(out=rt2[:], in_=rti[:]), ["posi"], ["xt1"])
                    V(lambda e: e.tensor_tensor(out=rt1[:], in0=rt1[:], in1=rt2[:], op=ALU.subtract), ["xt0", "xt1"], ["xt0"])
                    V(lambda e: e.scalar_tensor_tensor(out=rt2[:], in0=rt1[:], scalar=0.5, in1=rt1[:], op0=ALU.is_gt,
                                                       op1=ALU.subtract), ["xt0"], ["xt1"])
                    A(lambda e: e.activation(out=ropeC[:], in_=rt2[:], func=AF.Sin, scale=-SINSC), ["xt1"], ["ropeC"])
                    V(lambda e: e.tensor_scalar(out=rt1[:], in0=rt2[:], scalar1=-1.0, scalar2=0.25, op0=ALU.mult, op1=ALU.add),
                      ["xt1"], ["xt0"])
                    V(lambda e: e.scalar_tensor_tensor(out=rt2[:], in0=rt1[:], scalar=0.5, in1=rt1[:], op0=ALU.is_gt,
                                                       op1=ALU.subtract), ["xt0", "ropeC"], ["xt1"])
                    A(lambda e: e.activation(out=ropeD[:], in_=rt2[:], func=AF.Sin, scale=-SINSC), ["xt1"], ["ropeD"])

                def load_norm_T(x_src, row0, xbuf, kx):
                    LD(xbuf[:], x_src[row0:row0 + 128, :], [kx])
                    rmsnorm_stats(xbuf[:], ss[:], rstd[:], [kx], "ss", "rstd", D)
                    V(lambda e: e.scalar_tensor_tensor(out=xn[:], in0=xbuf[:], scalar=rstd[:, 0:1], in1=gmix[:],
                                                       op0=ALU.mult, op1=ALU.mult), [kx, "rstd", "gmix"], ["xn"])
                    def tr(e):
                        ins = None
                        for c in range(8):
                            ins = e.transpose(out=Ba[:, c, :], in_=xn[:, c * 128:(c + 1) * 128], identity=identb[:])
                        return ins
                    P(tr, ["xn", "identb"], ["Ba"])
                    A(lambda e: e.copy(out=xnT[:], in_=Ba[:]), ["Ba"], ["xnT"])

                def proj_fm(dst, dkey, col0, ntile):
                    def fn(e):
                        ins = None
                        for t in range(ntile):
                            c0 = col0 + 128 * t
                            for c in range(8):
                                ins = e.matmul(dst[:, t * 128:(t + 1) * 128], lhsT=win[:, c, c0:c0 + 128], rhs=xnT[:, c, :],
                                               start=(c == 0), stop=False)
                            ins = e.matmul(dst[:, t * 128:(t + 1) * 128], lhsT=bin_[0:1, c0:c0 + 128], rhs=onesb[0:1, :],
                                           start=False, stop=True)
                        return ins
                    return P(fn, ["win", "bin", "xnT", "onesb"], dkey)

                def proj_tm(dst, dkey, col0, n):
                    def fn(e):
                        ins = None
                        for c in range(8):
                            ins = e.matmul(dst, lhsT=xnT[:, c, :], rhs=win[:, c, col0:col0 + n], start=(c == 0), stop=False)
                        ins = e.matmul(dst, lhsT=onesb[0:1, :], rhs=bin_[0:1, col0:col0 + n], start=False, stop=True)
                        return ins
                    return P(fn, ["win", "bin", "xnT", "onesb"], dkey)

                def rope_apply(FA, FB, kA, kB, dst, dkey, toff, decoff):
                    Cb = ropeC[:, toff:toff + 128].unsqueeze(1).to_broadcast([128, 4, 128])
                    Db = ropeD[:, toff:toff + 128].unsqueeze(1).to_broadcast([128, 4, 128])
                    fa = FA[:].rearrange("p (h t) -> p h t", h=4)
                    fb = FB[:].rearrange("p (h t) -> p h t", h=4)
                    dc = dec[:, decoff:decoff + 512].rearrange("p (h t) -> p h t", h=4)
                    V(lambda e: e.tensor_tensor(out=ta[:], in0=fa, in1=Cb, op=ALU.mult), kA + ["ropeC"], ["ta"])
                    V(lambda e: e.tensor_tensor(out=tb[:], in0=fb, in1=Db, op=ALU.mult), kB + ["ropeD"], ["tb"])
                    V(lambda e: e.tensor_tensor(out=ta[:], in0=ta[:], in1=tb[:], op=ALU.add), ["ta", "tb"], ["ta"])
                    V(lambda e: e.tensor_tensor(out=dst[:], in0=ta[:], in1=dc, op=ALU.mult), ["ta", "dec"], [dkey])

                def ret_k_path(toff, par):
                    kT, khat, vret = kTs[par], khats[par], vrets[par]
                    proj_fm(F[0], ["F0"], O_KA, 4)
                    proj_fm(F[1], ["F1"], O_KB, 4)
                    rope_apply(F[0], F[1], ["F0"], ["F1"], kT, "kT%d" % par, toff, 512)
                    proj_tm(F[4][:, 0:512], ["F4"], O_RV, 512)
                    A(lambda e: e.copy(out=vret[:].rearrange("p h e -> p (h e)"), in_=F[4][:, 0:512]), ["F4"], ["vret%d" % par])
                    def tr(e):
                        ins = None
                        for h in range(4):
                            ins = e.transpose(out=Bb[:, h, :], in_=kT[:, h, :], identity=identb[:])
                        return ins
                    P(tr, ["kT%d" % par, "identb"], ["Bb"])
                    for h in range(4):
                        A(lambda e, h=h: e.mul(out=khat[:, h, :], in_=Bb[:, h, :], mul=CD[h]), ["Bb"], ["khat%d" % par])

                def state_update(par):
                    khat, vret = khats[par], vrets[par]
                    def fn(e):
                        ins = None
                        for h in range(4):
                            ins = e.matmul(F[4][:, h * 128:(h + 1) * 128], lhsT=khat[:, h, :], rhs=vret[:, h, :], start=True, stop=True)
                        return ins
                    P(fn, ["khat%d" % par, "vret%d" % par], ["F4"])
                    for h in range(4):
                        V(lambda e, h=h: e.scalar_tensor_tensor(out=Sst[:, h, :], in0=Sst[:, h, :], scalar=CD[h],
                                                                in1=F[4][:, h * 128:(h + 1) * 128], op0=ALU.mult, op1=ALU.add),
                          ["Sst", "F4"], ["Sst"])
                    A(lambda e: e.copy(out=Sbf[:], in_=Sst[:]), ["Sst"], ["Sbf"])

                def swa_kv(slot):
                    proj_fm(F[1], ["F1"], O_AKD, 2)
                    A(lambda e: e.copy(out=kTw[0:64, :, 0, slot, :], in_=F[1][0:64, 0:256].rearrange("p (k t) -> p k t", k=2)), ["F1"], ["kTw%d" % slot])
                    A(lambda e: e.copy(out=kTw[64:128, :, 1, slot, :], in_=F[1][64:128, 0:256].rearrange("p (k t) -> p k t", k=2)), ["F1", "kTw%d" % slot], ["kTw%d" % slot])
                    proj_tm(F[1][:, 256:384], ["F1"], O_AV, 128)
                    A(lambda e: e.copy(out=vsw[:, slot, :], in_=F[1][:, 256:384]), ["F1"], ["vsw%d" % slot])

                if st == 0:
                    def frontA(n):
                        toff = (n % 8) * 128
                        if n % 8 == 0 or n == 32 - n_prev:
                            make_rope(pos_prev[0:1, (n // 8) * 1024:(n // 8 + 1) * 1024])
                        load_norm_T(x_prev, n * 128, xt[n % 2], "xt%d" % (n % 2))
                        ret_k_path(toff, n % 2)
                        if n == 31:
                            swa_kv(2)
                    frontA(32 - n_prev)
                    for n in range(32 - n_prev, 32):
                        if n + 1 < 32 and (n + 1) % 8 != 0:
                            frontA(n + 1)
                        state_update(n % 2)
                        if n + 1 < 32 and (n + 1) % 8 == 0:
                            frontA(n + 1)
                    V(lambda e: e.tensor_scalar(out=Sst[:], in0=Sst[:], scalar1=flag[:, 0:1], scalar2=None, op0=ALU.mult),
                      ["Sst", "flag"], ["Sst"])
                    A(lambda e: e.copy(out=Sbf[:], in_=Sst[:]), ["Sst"], ["Sbf"])

                make_rope(pos_own[0:1, st * 1024:(st + 1) * 1024])

                def front(j):
                    n = st * 8 + j
                    par = n % 2
                    toff = j * 128
                    slot = n % 3
                    xb, kx = xt[n % 2], "xt%d" % (n % 2)
                    qT, sw, aqT = qTs[par], sws[par], aqTs[par]
                    load_norm_T(x_own, n * 128, xb, kx)
                    ret_k_path(toff, par)
                    proj_fm(F[2], ["F2"], O_QA, 4)
                    proj_fm(F[3], ["F3"], O_QB, 4)
                    rope_apply(F[2], F[3], ["F2"], ["F3"], qT, "qT%d" % par, toff, 0)
                    proj_tm(F[5][:, 0:512], ["F5"], O_RG, 512)
                    A(lambda e: e.activation(out=sw[:], in_=F[5][:, 0:512], func=AF.Silu), ["F5"], ["sw%d" % par])
                    V(lambda e: e.tensor_tensor(out=sw[:], in0=sw[:], in1=gret[:], op=ALU.mult), ["sw%d" % par, "gret"], ["sw%d" % par])
                    proj_fm(F[0], ["F0"], O_AQ, 4)
                    A(lambda e: e.copy(out=aqT[:].rearrange("p h t -> p (h t)"), in_=F[0][:, 0:512]), ["F0"], ["aqT%d" % par])
                    swa_kv(slot)

                def back(j):
                    n = st * 8 + j
                    par = n % 2
                    slot = n % 3
                    pslot = (n + 2) % 3
                    xb, kx = xt[n % 2], "xt%d" % (n % 2)
                    kT, qT, vret, sw, aqT = kTs[par], qTs[par], vrets[par], sws[par], aqTs[par]
                    kTk, qTk, vrk, swk, aqk = "kT%d" % par, "qT%d" % par, "vret%d" % par, "sw%d" % par, "aqT%d" % par
                    def fsc(e):
                        ins = None
                        for h in range(4):
                            ins = e.matmul(F[2][:, h * 128:(h + 1) * 128], lhsT=kT[:, h, :], rhs=qT[:, h, :], start=True, stop=True)
                        return ins
                    P(fsc, [kTk, qTk], ["F2"])
                    V(lambda e: e.tensor_tensor(out=PTr[:], in0=F[2][:].rearrange("p (h t) -> p h t", h=4),
                                                in1=causal[:].unsqueeze(1).to_broadcast([128, 4, 128]), op=ALU.mult),
                      ["F2", "causal"], ["PTr"])
                    def fio(e):
                        ins = None
                        for h in range(4):
                            e.matmul(F[3][:, h * 128:(h + 1) * 128], lhsT=PTr[:, h, :], rhs=vret[:, h, :], start=True, stop=False)
                            ins = e.matmul(F[3][:, h * 128:(h + 1) * 128], lhsT=qT[:, h, :], rhs=Sbf[:, h, :], start=False, stop=True)
                        return ins
                    P(fio, ["PTr", vrk, qTk, "Sbf"], ["F3"])
                    state_update(par)
                    f3 = F[3][:].rearrange("p (h t) -> p h t", h=4)
                    A(lambda e: e.activation(out=rsq[:], in_=f3, func=AF.Square), ["F3"], ["rsq"])
                    V(lambda e: e.tensor_reduce(out=rss[:], in_=rsq[:], axis=AX.X, op=ALU.add), ["rsq"], ["rss"])
                    A(lambda e: e.activation(out=rrs[:], in_=rss[:], func=AF.Ln, bias=epst[:, 0:1], scale=1.0 / 128), ["rss", "epst"], ["rrs"])
                    A(lambda e: e.activation(out=rrs[:], in_=rrs[:], func=AF.Exp, scale=-0.5), ["rrs"], ["rrs"])
                    V(lambda e: e.tensor_tensor(out=rt[:], in0=f3, in1=rrs[:].unsqueeze(2).to_broadcast([128, 4, 128]), op=ALU.mult),
                      ["F3", "rrs"], ["rt"])
                    V(lambda e: e.tensor_tensor(out=cat[:, 0:512], in0=rt[:].rearrange("p h t -> p (h t)"), in1=sw[:], op=ALU.mult),
                      ["rt", swk], ["cat_r"])
                    for hh in range(2):
                        for pr in range(2):
                            bank, bkey = (F[5], "F5") if pr == 0 else (F[0], "F0")
                            def fqk(e, hh=hh, pr=pr, bank=bank, slot=slot, pslot=pslot):
                                ins = None
                                for i2 in range(2):
                                    h = hh * 4 + pr * 2 + i2
                                    jp, i, kvh = h // 2, h % 2, h // 4
                                    bo = i2 * 256
                                    e.matmul(bank[:, bo:bo + 128], lhsT=aqT[:, jp, :], rhs=kTw[:, kvh, i, pslot, :], start=True, stop=True)
                                    ins = e.matmul(bank[:, bo + 128:bo + 256], lhsT=aqT[:, jp, :], rhs=kTw[:, kvh, i, slot, :], start=True, stop=True)
                                return ins
                            P(fqk, [aqk, "kTw%d" % slot, "kTw%d" % pslot], [bkey])
                            for i2 in range(2):
                                hl = pr * 2 + i2
                                h = hh * 4 + hl
                                bo = i2 * 256
                                V(lambda e, h=h, hl=hl, bank=bank, bo=bo: e.scalar_tensor_tensor(out=sc[:, hl, :], in0=bank[:, bo:bo + 256], scalar=0.125,
                                                                                                in1=biassw[:, h, :], op0=ALU.mult, op1=ALU.add),
                                  [bkey, "biassw"], ["sc%d" % hl])
                                if n == 0:
                                    V(lambda e, hl=hl: e.tensor_scalar(out=sc[:, hl, 0:128], in0=sc[:, hl, 0:128], scalar1=flag[:, 1:2], scalar2=None,
                                                                       op0=ALU.add), ["sc%d" % hl, "flag"], ["sc%d" % hl])
                        sck = ["sc%d" % hl for hl in range(4)]
                        sk4 = sinks[:, hh * 4:(hh + 1) * 4]
                        if stop_after == "dbg_sc" and hh == 0:
                            V(lambda e, j=j: e.tensor_copy(out=acc[:, j, :], in_=sc[:].rearrange("p h k -> p (h k)")), sck, ["acc%d" % j])
                        V(lambda e: e.tensor_reduce(out=rmax[:, 0:4], in_=sc[:], axis=AX.X, op=ALU.max), sck, ["rmax"])
                        V(lambda e, sk4=sk4: e.tensor_tensor(out=rmax[:, 0:4], in0=rmax[:, 0:4], in1=sk4, op=ALU.max), ["rmax", "sinks"], ["rmax"])
                        V(lambda e: e.tensor_scalar(out=nmax[:, 0:4], in0=rmax[:, 0:4], scalar1=-1.0, scalar2=None, op0=ALU.mult), ["rmax"], ["nmax"])
                        V(lambda e, sk4=sk4: e.tensor_tensor(out=esk[:, 0:4], in0=sk4, in1=rmax[:, 0:4], op=ALU.subtract), ["rmax", "sinks"], ["esk"])
                        A(lambda e: e.activation(out=esk[:, 0:4], in_=esk[:, 0:4], func=AF.Exp), ["esk"], ["esk"])
                        for hl in range(4):
                            A(lambda e, hl=hl: e.activation(out=Pb[:, hl, :], in_=sc[:, hl, :], func=AF.Exp, bias=nmax[:, hl:hl + 1],
                                                            accum_out=rsum[:, hl:hl + 1]), ["sc%d" % hl, "nmax"], ["Pb%d" % hl, "rsum%d" % hl])
                        V(lambda e: e.tensor_tensor(out=rden[:, 0:4], in0=rsum[:, 0:4], in1=esk[:, 0:4], op=ALU.add),
                          ["rsum%d" % hl for hl in range(4)] + ["esk"], ["rden"])
                        V(lambda e, hh=hh: e.reciprocal(out=rden8[:, hh * 4:(hh + 1) * 4], in_=rden[:, 0:4]), ["rden"], ["rden8_%d" % hh])
                        def ftp(e):
                            ins = None
                            for hl in range(4):
                                e.transpose(out=Bb[:, 2 * hl, :], in_=Pb[:, hl, 0:128], identity=identb[:])
                                ins = e.transpose(out=Bb[:, 2 * hl + 1, :], in_=Pb[:, hl, 128:256], identity=identb[:])
                            return ins
                        P(ftp, ["Pb%d" % hl for hl in range(4)] + ["identb"], ["Bb"])
                        A(lambda e: e.copy(out=PTs[:], in_=Bb[:]), ["Bb"], ["PTs"])
                        def fpv(e, hh=hh, slot=slot, pslot=pslot):
                            ins = None
                            for hl in range(4):
                                h = hh * 4 + hl
                                kvh = h // 4
                                e.matmul(F[1][:, h * 64:(h + 1) * 64], lhsT=PTs[:, 2 * hl, :], rhs=vsw[:, pslot, kvh * 64:(kvh + 1) * 64],
                                         start=True, stop=False)
                                ins = e.matmul(F[1][:, h * 64:(h + 1) * 64], lhsT=PTs[:, 2 * hl + 1, :], rhs=vsw[:, slot, kvh * 64:(kvh + 1) * 64],
                                               start=False, stop=True)
                            return ins
                        P(fpv, ["PTs", "vsw%d" % slot, "vsw%d" % pslot], ["F1"])
                    V(lambda e: e.tensor_tensor(out=cat[:, 512:1024].rearrange("p (h d) -> p h d", h=8),
                                                in0=F[1][:].rearrange("p (h d) -> p h d", h=8),
                                                in1=rden8[:].unsqueeze(2).to_broadcast([128, 8, 64]), op=ALU.mult),
                      ["F1", "rden8_0", "rden8_1"], ["cat_a"])
                    def trc(e):
                        ins = None
                        for c in range(8):
                            ins = e.transpose(out=Ba[:, c, :], in_=cat[:, c * 128:(c + 1) * 128], identity=identb[:])
                        return ins
                    P(trc, ["cat_r", "cat_a", "identb"], ["Ba"])
                    A(lambda e: e.copy(out=catT[:], in_=Ba[:]), ["Ba"], ["catT"])
                    if stop_after == "cat":
                        V(lambda e, j=j: e.tensor_copy(out=acc[:, j, :], in_=cat[:]), ["cat_r", "cat_a"], ["acc%d" % j])
                    for half in range(2):
                        if stop_after in ("cat", "dbg_sc"):
                            break
                        bank, bk = (F[2], "F2") if half == 0 else (F[4], "F4")
                        mm(bank[:, 0:512], [(catT[:, c, :], wout[:, c, half * 512:(half + 1) * 512]) for c in range(8)],
                           ["catT", "wout"], [bk])
                        V(lambda e, half=half, bank=bank, j=j, xb=xb: e.tensor_tensor(out=acc[:, j, half * 512:(half + 1) * 512], in0=bank[:, 0:512],
                                                                                  in1=xb[:, half * 512:(half + 1) * 512], op=ALU.add),
                          [bk, kx], ["acc%d" % j])

                front(0)
                for j in range(8):
                    if j + 1 < 8:
                        front(j + 1)
                    back(j)
                S.barrier()
            if stop_after in ("mixer", "cat", "dbg_sc"):
                for j in range(8):
                    out_ops.append(S.op("sp", lambda e, j=j: [e.dma_start(out=out_d[(st * 8 + j) * 128:(st * 8 + j + 1) * 128, :], in_=acc[:, j, :])],
                                        ["acc%d" % j], [], dma=True, key=("o", j), force=True))
                S.barrier()
                continue

            with contextlib.ExitStack() as es2:
                sb_top[0], ps_top[0] = sb_mark, 0
                xT = sb(es2, "xT", [128, 8, 1024], BF16)
                xnf = sb(es2, "xnf", [128, D]); xh = sb(es2, "xh", [128, D], BF16); xl = sb(es2, "xl", [128, D], BF16)
                xTls = [sb(es2, "xTl%d" % i, [128, 8, 128], BF16) for i in range(2)]
                wrt = sb(es2, "wrt", [128, 8, 32]); wrh = sb(es2, "wrh", [128, 8, 32], BF16); wrl = sb(es2, "wrl", [128, 8, 32], BF16)
                wrd = sb(es2, "wrd", [128, 8, 32])
                BT = ps(es2, "BT", [128, 8, 128], BF16)
                lg = sb(es2, "lg", [128, 32]); t8 = sb(es2, "t8", [128, 8]); msk = sb(es2, "msk", [128, 32])
                ex = sb(es2, "ex", [128, 32]); nm1 = sb(es2, "nm1", [128, 1]); gs = sb(es2, "gs", [128, 1])
                G = sb(es2, "G", [128, 8, 32])
                ss2 = sb(es2, "ss2", [128, 1]); rstd2 = sb(es2, "rstd2", [128, 1])
                bup = sb(es2, "bup", [128, NEXP * 16])
                bdn = sb(es2, "bdn", [32, D], BF16)
                Gb = sb(es2, "Gb", [128, 32], BF16); GT = sb(es2, "GT", [32, 128], BF16)
                wu = [sb(es2, "wu%d" % i, [128, 8, 1024], BF16) for i in range(2)]
                wd = [sb(es2, "wd%d" % i, [128, 4, D], BF16) for i in range(2)]
                aT = [sb(es2, "aT%d" % i, [128, 4, 512], BF16) for i in range(2)]
                gg = [sb(es2, "gg%d" % i, [128, 512]) for i in range(2)]
                sg = [sb(es2, "sg%d" % i, [128, 512]) for i in range(2)]
                ll = [sb(es2, "ll%d" % i, [128, 512]) for i in range(2)]
                UG = [ps(es2, "UG%d" % i, [128, 512]) for i in range(2)]
                UL = [ps(es2, "UL%d" % i, [128, 512]) for i in range(2)]
                DNt = ps(es2, "DNt", [128, 1536])
                DN = [DNt[:, 0:512], DNt[:, 512:1024], DNt[:, 1024:1536]]
                PL = DN[2]
                gmoe = sb(es2, "gmoe", [128, D])
                LD(gmoe[:], g_moe_d[0:1, :].partition_broadcast(128), ["gmoe"])

                LD(wrt[:], w_rt_d.rearrange("(c p) n -> p c n", p=128), ["wrt"])
                V(lambda e: e.tensor_copy(out=wrh[:], in_=wrt[:]), ["wrt"], ["wrh"])
                V(lambda e: e.tensor_tensor(out=wrd[:], in0=wrt[:], in1=wrh[:], op=ALU.subtract), ["wrt", "wrh"], ["wrd"])
                V(lambda e: e.tensor_copy(out=wrl[:], in_=wrd[:]), ["wrd"], ["wrl"])
                LD(bup[:], b_up_d[:, :], ["bup"])
                LD(bdn[:], b_dn_d.rearrange("o (e d) -> (o e) d", e=32), ["bdn"], eng="pool")
                bup3 = bup[:].rearrange("p (e c) -> p e c", c=16)
                V(lambda e: e.tensor_scalar(out=bup3[:, :, 8:16], in0=bup3[:, :, 8:16], scalar1=1.0, scalar2=None, op0=ALU.add), ["bup"], ["bup"])

                def rA(j):
                    ak = "acc%d" % j
                    par = j % 2
                    xTl_ = xTls[par]
                    rmsnorm_stats(acc[:, j, :], ss2[:], rstd2[:], [ak], "ss2", "rstd2", D)
                    V(lambda e: e.scalar_tensor_tensor(out=xnf[:], in0=acc[:, j, :], scalar=rstd2[:, 0:1], in1=gmoe[:],
                                                       op0=ALU.mult, op1=ALU.mult), [ak, "rstd2", "gmoe"], ["xnf"])
                    V(lambda e: e.tensor_copy(out=xh[:], in_=xnf[:]), ["xnf"], ["xh"])
                    V(lambda e: e.tensor_tensor(out=xl[:], in0=xnf[:], in1=xh[:], op=ALU.subtract), ["xnf", "xh"], ["xl"])
                    def trh_(e):
                        ins = None
                        for c in range(8):
                            ins = e.transpose(out=BT[:, c, :], in_=xh[:, c * 128:(c + 1) * 128], identity=identb[:])
                        return ins
                    P(trh_, ["xh", "identb"], ["BT"])
                    A(lambda e: e.copy(out=xT[:, :, j * 128:(j + 1) * 128], in_=BT[:]), ["BT"], ["xT%d" % j])
                    def trl_(e):
                        ins = None
                        for c in range(8):
                            ins = e.transpose(out=BT[:, c, :], in_=xl[:, c * 128:(c + 1) * 128], identity=identb[:])
                        return ins
                    P(trl_, ["xl", "identb"], ["BT"])
                    V(lambda e: e.tensor_copy(out=xTl_[:], in_=BT[:]), ["BT"], ["xTl%d" % par])

                def rB(j):
                    par = j % 2
                    xTl_ = xTls[par]
                    mm(PL[:, 0:32], [(xT[:, c, j * 128:(j + 1) * 128], wrh[:, c, :]) for c in range(8)]
                       + [(xTl_[:, c, :], wrh[:, c, :]) for c in range(8)]
                       + [(xT[:, c, j * 128:(j + 1) * 128], wrl[:, c, :]) for c in range(8)], ["xT%d" % j, "xTl%d" % par, "wrh", "wrl"], ["DN2"])
                    V(lambda e: e.tensor_tensor(out=lg[:], in0=PL[:, 0:32], in1=brt[:], op=ALU.add), ["DN2", "brt"], ["lg"])
                    V(lambda e: e.max(out=t8[:], in_=lg[:]), ["lg"], ["t8"])
                    V(lambda e: e.tensor_scalar(out=msk[:], in0=lg[:], scalar1=t8[:, 3:4], scalar2=None, op0=ALU.is_ge), ["lg", "t8"], ["msk"])
                    V(lambda e: e.tensor_scalar(out=nm1[:], in0=t8[:, 0:1], scalar1=-1.0, scalar2=None, op0=ALU.mult), ["t8"], ["nm1"])
                    A(lambda e: e.activation(out=ex[:], in_=lg[:], func=AF.Exp, bias=nm1[:, 0:1]), ["lg", "nm1"], ["ex"])
                    V(lambda e: e.tensor_tensor(out=ex[:], in0=ex[:], in1=msk[:], op=ALU.mult), ["ex", "msk"], ["ex"])
                    V(lambda e: e.tensor_reduce(out=gs[:], in_=ex[:], axis=AX.X, op=ALU.add), ["ex"], ["gs"])
                    V(lambda e: e.reciprocal(out=gs[:], in_=gs[:]), ["gs"], ["gs"])
                    V(lambda e: e.tensor_scalar(out=G[:, j, :], in0=ex[:], scalar1=gs[:, 0:1], scalar2=None, op0=ALU.mult),
                      ["ex", "gs"], ["G%d" % j])
                    V(lambda e: e.tensor_copy(out=Gb[:], in_=G[:, j, :]), ["G%d" % j], ["Gb"])
                    P(lambda e: e.transpose(out=BT[0:32, 0, :], in_=Gb[:, :], identity=identb[:]), ["Gb", "identb"], ["BT"])
                    A(lambda e: e.copy(out=GT[:], in_=BT[0:32, 0, :]), ["BT"], ["GT"])
                    for dh in range(2):
                        mm(DN[dh], [(GT[:, :], bdn[:, dh * 512:(dh + 1) * 512])], ["GT", "bdn"], ["DN%d" % dh])
                        V(lambda e, dh=dh: e.tensor_tensor(out=acc[:, j, dh * 512:(dh + 1) * 512], in0=DN[dh],
                                                           in1=acc[:, j, dh * 512:(dh + 1) * 512], op=ALU.add),
                          ["DN%d" % dh, "acc%d" % j], ["acc%d" % j])

                rA(0)
                for j in range(8):
                    if j + 1 < 8:
                        rA(j + 1)
                    rB(j)

                xTk = ["xT%d" % j for j in range(8)]
                u = 0
                for ex_i in range(n_exp):
                    for hf in range(2):
                        slot = u % 2
                        u += 1
                        wuk, wdk = "wu%d" % slot, "wd%d" % slot
                        f0 = hf * 512
                        S.op("pool", lambda e, slot=slot, ex_i=ex_i, f0=f0: [
                            e.dma_start(out=wu[slot][:, :, 0:512], in_=w_up_d[ex_i, :, f0:f0 + 512].rearrange("(c p) n -> p c n", p=128)),
                            e.dma_start(out=wu[slot][:, :, 512:1024], in_=w_up_d[ex_i, :, 1024 + f0:1024 + f0 + 512].rearrange("(c p) n -> p c n", p=128)),
                        ], [], [wuk], dma=True, ndma=2)
                        S.op("pool", lambda e, slot=slot, ex_i=ex_i, f0=f0: [
                            e.dma_start(out=wd[slot][:], in_=w_dn_d[ex_i, f0:f0 + 512, :].rearrange("(c p) n -> p c n", p=128)),
                        ], [], [wdk], dma=True)
                        def up(g):
                            a_s = g % 2
                            ak_ = "aT%d" % a_s
                            for fc in range(4):
                                b_ = fc % 2
                                cg = hf * 4 + fc
                                mm(UG[b_][:], [(wu[slot][:, c, fc * 128:(fc + 1) * 128], xT[:, c, g * 512:(g + 1) * 512]) for c in range(8)],
                                   [wuk] + xTk[g * 4:(g + 1) * 4], ["UG%d" % b_])
                                mm(UL[b_][:], [(wu[slot][:, c, 512 + fc * 128:512 + (fc + 1) * 128], xT[:, c, g * 512:(g + 1) * 512]) for c in range(8)],
                                   [wuk] + xTk[g * 4:(g + 1) * 4], ["UL%d" % b_])
                                bg = bup[:, ex_i * 16 + cg:ex_i * 16 + cg + 1]
                                bl = bup[:, ex_i * 16 + 8 + cg:ex_i * 16 + 8 + cg + 1]
                                V(lambda e, b_=b_, bg=bg: e.tensor_scalar(out=gg[b_][:], in0=UG[b_][:], scalar1=bg, scalar2=7.0, op0=ALU.add, op1=ALU.min),
                                  ["UG%d" % b_, "bup"], ["gg%d" % b_])
                                A(lambda e, b_=b_: e.activation(out=sg[b_][:], in_=gg[b_][:], func=AF.Sigmoid, scale=1.702), ["gg%d" % b_], ["sg%d" % b_])
                                V(lambda e, b_=b_, bl=bl: e.tensor_scalar(out=ll[b_][:], in0=UL[b_][:], scalar1=bl, scalar2=-6.0, op0=ALU.add, op1=ALU.max),
                                  ["UL%d" % b_, "bup"], ["ll%d" % b_])
                                V(lambda e, b_=b_: e.scalar_tensor_tensor(out=ll[b_][:], in0=ll[b_][:], scalar=8.0, in1=gg[b_][:], op0=ALU.min, op1=ALU.mult),
                                  ["ll%d" % b_, "gg%d" % b_], ["ll%d" % b_])
                                V(lambda e, b_=b_, a_s=a_s, fc=fc: e.tensor_tensor(out=aT[a_s][:, fc, :], in0=ll[b_][:], in1=sg[b_][:], op=ALU.mult),
                                  ["ll%d" % b_, "sg%d" % b_], [ak_ + "_%d" % fc])

                        def down(g):
                            a_s = g % 2
                            ak_ = "aT%d" % a_s
                            for tt in range(4):
                                j = g * 4 + tt
                                for dh in range(2):
                                    pairs = [(aT[a_s][:, fc, tt * 128:(tt + 1) * 128], wd[slot][:, fc, dh * 512:(dh + 1) * 512]) for fc in range(4)]
                                    db = (2 * tt + dh) % 3
                                    mm(DN[db], pairs, [ak_ + "_%d" % fc for fc in range(4)] + [wdk], ["DN%d" % db])
                                    V(lambda e, j=j, dh=dh, db=db, ex_i=ex_i: e.scalar_tensor_tensor(out=acc[:, j, dh * 512:(dh + 1) * 512], in0=DN[db],
                                                                                             scalar=G[:, j, ex_i:ex_i + 1], in1=acc[:, j, dh * 512:(dh + 1) * 512],
                                                                                             op0=ALU.mult, op1=ALU.add),
                                      ["DN%d" % db, "G%d" % j, "acc%d" % j], ["acc%d" % j])

                        up(0); up(1); down(0); down(1)
                S.barrier()
            if stop_after == "moe":
                for j in range(8):
                    out_ops.append(S.op("sp", lambda e, j=j: [e.dma_start(out=out_d[(st * 8 + j) * 128:(st * 8 + j + 1) * 128, :], in_=acc[:, j, :])],
                                        ["acc%d" % j], [], dma=True, key=("o", j), force=True))
                S.barrier()
                continue

            with contextlib.ExitStack() as es3:
                sb_top[0], ps_top[0] = sb_mark, 0
                wpg = sb(es3, "wpg", [128, 8, D], BF16); wpp = sb(es3, "wpp", [128, 2, D], BF16)
                pTs = sb(es3, "pTs", [128, 2, 1024], BF16)
                hb = sb(es3, "hb", [128, D], BF16); hTs = [sb(es3, "hT%d" % i, [128, 8, 128], BF16) for i in range(2)]
                gate = sb(es3, "gate", [128, D]); prod = sb(es3, "prod", [128, D])
                ss3 = sb(es3, "ss3", [128, 1]); rstd3 = sb(es3, "rstd3", [128, 1])
                ob = [sb(es3, "ob%d" % i, [128, D]) for i in range(2)]
                Bc = ps(es3, "Bc", [128, 8, 128], BF16)
                PG = [ps(es3, "PG%d" % i, [128, 512]) for i in range(2)]
                PP = [ps(es3, "PP%d" % i, [128, 512]) for i in range(2)]
                gple = sb(es3, "gple", [128, D]); gfin = sb(es3, "gfin", [128, D])
                LD(gple[:], g_ple_d[0:1, :].partition_broadcast(128), ["gple"])
                LD(gfin[:], g_fin_d[0:1, :].partition_broadcast(128), ["gfin"])
                LD(wpg[:], w_pg_d.rearrange("(c p) n -> p c n", p=128), ["wpg"], eng="pool")
                LD(wpp[:], w_pp_d.rearrange("(c p) n -> p c n", p=128), ["wpp"], eng="pool")
                LD(pTs[:], pT_d[:, st * 1024:(st + 1) * 1024].rearrange("(c p) n -> p c n", p=128), ["pTs"], eng="pool")
                assert sb_top[0] <= W_TOP, sb_top[0]
                if st + 1 < n_super:
                    load_mixer_weights()

                def pfront(j):
                    hT = hTs[j % 2]
                    A(lambda e: e.copy(out=hb[:], in_=acc[:, j, :]), ["acc%d" % j], ["hb"])
                    def trh(e):
                        ins = None
                        for c in range(8):
                            ins = e.transpose(out=Bc[:, c, :], in_=hb[:, c * 128:(c + 1) * 128], identity=identb[:])
                        return ins
                    P(trh, ["hb", "identb"], ["Bc"])
                    A(lambda e: e.copy(out=hT[:], in_=Bc[:]), ["Bc"], ["hT%d" % (j % 2)])

                def pback(j):
                    ak = "acc%d" % j
                    hT, hk = hTs[j % 2], "hT%d" % (j % 2)
                    o_ = ob[j % 2]; ok = "ob%d" % (j % 2)
                    for dh in range(2):
                        mm(PG[dh][:], [(hT[:, c, :], wpg[:, c, dh * 512:(dh + 1) * 512]) for c in range(8)], [hk, "wpg"], ["PG%d" % dh])
                        mm(PP[dh][:], [(pTs[:, c, j * 128:(j + 1) * 128], wpp[:, c, dh * 512:(dh + 1) * 512]) for c in range(2)], ["pTs", "wpp"], ["PP%d" % dh])
                        A(lambda e, dh=dh: e.activation(out=gate[:, dh * 512:(dh + 1) * 512], in_=PG[dh][:], func=AF.Sigmoid), ["PG%d" % dh], ["gate%d" % dh])
                        V(lambda e, dh=dh: e.tensor_tensor(out=prod[:, dh * 512:(dh + 1) * 512], in0=PP[dh][:], in1=gate[:, dh * 512:(dh + 1) * 512], op=ALU.mult),
                          ["PP%d" % dh, "gate%d" % dh], ["prod%d" % dh])
                    rmsnorm_stats(prod[:], ss3[:], rstd3[:], ["prod0", "prod1"], "ss3", "rstd3", D)
                    V(lambda e: e.scalar_tensor_tensor(out=prod[:], in0=prod[:], scalar=rstd3[:, 0:1], in1=gple[:], op0=ALU.mult, op1=ALU.mult),
                      ["prod0", "prod1", "rstd3", "gple"], ["prod0", "prod1"])
                    V(lambda e: e.tensor_tensor(out=prod[:], in0=prod[:], in1=acc[:, j, :], op=ALU.add), ["prod0", "prod1", ak], ["prod0", "prod1"])
                    rmsnorm_stats(prod[:], ss3[:], rstd3[:], ["prod0", "prod1"], "ss3", "rstd3", D)
                    V(lambda e: e.scalar_tensor_tensor(out=o_[:], in0=prod[:], scalar=rstd3[:, 0:1], in1=gfin[:], op0=ALU.mult, op1=ALU.mult),
                      ["prod0", "prod1", "rstd3", "gfin"], [ok])
                    r0 = (st * 8 + j) * 128
                    out_ops.append(S.op("sp", lambda e: [e.dma_start(out=out_d[r0:r0 + 128, :], in_=o_[:])], [ok], [], dma=True, key=("o", j % 2)))

                pfront(0)
                for j in range(8):
                    if j + 1 < 8:
                        pfront(j + 1)
                    pback(j)
                S.barrier()
        S.emit(final_wait_ops=out_ops)
    return nc


def _t5_bucket(n):
    n = np.asarray(n)
    max_exact = 16
    nf = np.maximum(n, 1).astype(np.float32)
    large = max_exact + (np.log(nf / max_exact) / math.log(128 / max_exact) * (32 - max_exact)).astype(np.int32)
    large = np.minimum(large, 31)
    return np.where(n < max_exact, n, large)


def _host_consts():
    c = {}
    qi = np.arange(128)[:, None]; ki = np.arange(256)[None, :]
    dist = qi + 128 - ki
    c["band_ok"] = (dist >= 0) & (dist < 128)
    c["bucket"] = _t5_bucket(np.maximum(dist, 0))
    half = 64
    inv = (10000.0 ** (-np.arange(half, dtype=np.float32) / half)).astype(np.float32)
    ropec = np.zeros((128, 2), np.float32)
    ropec[:, 0] = np.concatenate([inv, inv]) / np.float32(2 * math.pi)
    ropec[:64, 1] = 0.25
    c["ropec"] = ropec
    dec = np.zeros((1, 1024), np.float32)
    for h in range(4):
        lg = math.log(GAM[h])
        dec[0, h * 128:(h + 1) * 128] = np.exp((np.arange(128) + 1) * lg)
        dec[0, 512 + h * 128:512 + (h + 1) * 128] = np.exp(-(np.arange(128) + 1) * lg) * (128 ** -0.5)
    c["dec"] = dec
    c["causal"] = (np.arange(128)[None, :] >= np.arange(128)[:, None]).astype(np.float32)
    c["ident"] = np.eye(128, dtype=np.float32)
    cols = []
    for sec in (512 + 0, 512 + 64, 0, 64):
        for h in range(4):
            b = sec + h * 128
            cols += list(range(b, b + 64)) * 2
    cols += list(range(1024, 1536)) + list(range(1536, 2048)) + list(range(2048, 2560))
    for kvh in range(2):
        cols += list(range(2560 + kvh * 64, 2560 + kvh * 64 + 64)) * 2
    cols += list(range(2688, 2816))
    c["cols"] = np.array(cols)
    assert len(cols) == NCOL
    return c


_PROG = {}


def kernel(x, p, positions, rel_bias_table, g_mix_norm, w_in, b_in, ret_norm_g, att_sinks, w_out, g_moe_norm,
           w_router, b_router, w_up, b_up, w_down, b_down, w_ple_gate, w_ple_proj, g_ple_norm, g_final, _build_kw=None):
    f = lambda a: np.ascontiguousarray(np.asarray(a, dtype=np.float32))
    x = f(x); p = f(p); positions = np.ascontiguousarray(np.asarray(positions, dtype=np.int32))
    hc = _host_consts()
    tab = f(rel_bias_table)
    bias = tab[hc["bucket"]]
    bias = np.where(hc["band_ok"][:, :, None], bias, np.float32(-30000.0))
    bias_sw = np.ascontiguousarray(np.transpose(bias, (0, 2, 1))).reshape(128, 8 * 256)
    w_in_r = np.ascontiguousarray(f(w_in)[0][:, hc["cols"]])
    b_in_r = np.ascontiguousarray(f(b_in)[0][hc["cols"]])[None, :]
    wu = f(w_up)[0]
    w_up_r = np.ascontiguousarray(np.concatenate([wu[:, :, 0::2], wu[:, :, 1::2]], axis=2))
    bu = f(b_up)[0]
    bu_r = np.concatenate([bu[:, 0::2], bu[:, 1::2]], axis=1)
    b_up_fm = np.ascontiguousarray(bu_r.reshape(32, 16, 128).transpose(2, 0, 1)).reshape(128, 32 * 16)
    common = {
        "bias_sw": bias_sw, "w_in_r": w_in_r, "b_in_r": b_in_r, "ropec": hc["ropec"], "dec": hc["dec"],
        "causal": hc["causal"], "ident": hc["ident"], "g_mix": f(g_mix_norm)[0][None, :],
        "g_ret": f(ret_norm_g)[0].reshape(1, 512), "sinks": f(att_sinks)[0][None, :], "w_out": f(w_out)[0],
        "g_moe": f(g_moe_norm)[0][None, :], "w_router": f(w_router)[0], "b_router": f(b_router)[0][None, :],
        "w_up_r": w_up_r, "b_up_fm": b_up_fm, "w_down": f(w_down)[0], "b_down": f(b_down)[0].reshape(1, 32 * D),
        "w_ple_gate": f(w_ple_gate)[0], "w_ple_proj": f(w_ple_proj)[0], "g_ple": f(g_ple_norm)[0][None, :],
        "g_final": f(g_final)[None, :],
    }
    in_maps = []
    for c in range(NCORE):
        b, hf = c // 2, c % 2
        lo = hf * TOK
        m = dict(common)
        m["x_own"] = np.ascontiguousarray(x[b, lo:lo + TOK])
        m["x_prev"] = np.ascontiguousarray(x[b, 0:TOK]) if hf == 1 else np.zeros((TOK, D), np.float32)
        m["pos_own"] = np.ascontiguousarray(positions[b, lo:lo + TOK])[None, :]
        m["pos_prev"] = np.ascontiguousarray(positions[b, 0:TOK])[None, :]
        m["pT"] = np.ascontiguousarray(p[0, b, lo:lo + TOK].T)
        fl = np.zeros((128, 2), np.float32)
        fl[:, 0] = 1.0 if hf == 1 else 0.0
        fl[:, 1] = 0.0 if hf == 1 else -30000.0
        m["flag"] = fl
        in_maps.append(m)
    kw = _build_kw or {}
    ne = kw.get("n_exp", NEXP)
    if ne != NEXP:
        for m in in_maps:
            m["w_up_r"] = np.ascontiguousarray(m["w_up_r"][:ne]); m["w_down"] = np.ascontiguousarray(m["w_down"][:ne])
    key = tuple(sorted(kw.items()))
    if key not in _PROG:
        _PROG[key] = build_program(**kw)
    nc = _PROG[key]
    res = run_bass_kernel_spmd(nc, in_maps, core_ids=list(range(NCORE)))
    out = np.zeros((4, 8192, D), np.float32)
    for c in range(NCORE):
        b, hf = c // 2, c % 2
        out[b, hf * TOK:(hf + 1) * TOK] = res.results[c]["out"]
    return out
```
